# Optimizing a Trainium2 kernel written in Bass

```python
import math
import jax
import jax.numpy as jnp
from jax import lax
import numpy as np

D_MODEL = 4096
BATCH = 2
SEQ = 8192
DEPTH = 2

CHUNK = 64
LRU_WIDTH = D_MODEL // 2
LRU_BLOCKS = 16
LRU_BLOCK = LRU_WIDTH // LRU_BLOCKS
CONV_WIDTH = 4
LRU_C = 8.0
HEAD_DIM = 128
ATT_WIDTH = D_MODEL // 4
ATT_HEADS = ATT_WIDTH // HEAD_DIM
LEFT_CHUNKS = 8
BAND_CHUNKS = LEFT_CHUNKS + 1
BAND = BAND_CHUNKS * CHUNK
REL_CLIP = 128
SSM_WIDTH = D_MODEL // 4
SSM_GROUP = 16
SSM_GROUPS = SSM_WIDTH // SSM_GROUP
SSM_STATE = 64
N_BRANCHES = 3
SPLIT_POINTS = (LRU_WIDTH, LRU_WIDTH + ATT_WIDTH, LRU_WIDTH + 2 * ATT_WIDTH, LRU_WIDTH + 3 * ATT_WIDTH, LRU_WIDTH + 3 * ATT_WIDTH + SSM_WIDTH)
IN_WIDTH = LRU_WIDTH + 3 * ATT_WIDTH + SSM_WIDTH + N_BRANCHES * D_MODEL
MOE_GROUPS = 4
EXPERTS_PER_GROUP = 8
N_EXPERTS = MOE_GROUPS * EXPERTS_PER_GROUP
TOP_K = 2
EXPERT_FF = D_MODEL // 8
PLE_DIM = 256
EPS = 1e-6
NEG_INF = -1e30

kernel_name = 'hybrid_rglru_chunkattn_s5_hmoe_block'


def _rms32(x, gain):
    x32 = x.astype(jnp.float32)
    y = x32 * lax.rsqrt(jnp.mean(x32 * x32, axis=-1, keepdims=True) + EPS)
    return y * gain.astype(jnp.float32)


def rms_norm(x, gain):
    return _rms32(x, gain).astype(x.dtype)


def rglru_branch(u, conv_w, conv_b, w_rg, b_rg, w_ig, b_ig, lam):
    bsz, L, _ = u.shape
    up = jnp.pad(u, ((0, 0), (CONV_WIDTH - 1, 0), (0, 0)))
    xc = conv_b
    for j in range(CONV_WIDTH):
        xc = xc + conv_w[j] * up[:, j:j + L]
    xb = xc.reshape(bsz, L, LRU_BLOCKS, LRU_BLOCK)
    r = jax.nn.sigmoid((jnp.einsum('blhi,hij->blhj', xb, w_rg) + b_rg).astype(jnp.float32)).reshape(bsz, L, LRU_WIDTH)
    ig = jax.nn.sigmoid((jnp.einsum('blhi,hij->blhj', xb, w_ig) + b_ig).astype(jnp.float32)).reshape(bsz, L, LRU_WIDTH)
    log_a = -LRU_C * r * jax.nn.softplus(-lam.astype(jnp.float32))
    a = jnp.exp(log_a)
    xin = jnp.sqrt(-jnp.expm1(2.0 * log_a)) * (ig * xc.astype(jnp.float32))

    def step(h, inp):
        a_t, x_t = inp
        h = a_t * h + x_t
        return h, h

    h0 = jnp.zeros((bsz, LRU_WIDTH), jnp.float32)
    _, hs = lax.scan(step, h0, (jnp.swapaxes(a, 0, 1), jnp.swapaxes(xin, 0, 1)))
    return jnp.swapaxes(hs, 0, 1).astype(u.dtype)


def _band(t, n_chunks):
    bsz = t.shape[0]
    tc = t.reshape(bsz, n_chunks, CHUNK, ATT_HEADS, HEAD_DIM)
    tp = jnp.pad(tc, ((0, 0), (LEFT_CHUNKS, 0), (0, 0), (0, 0), (0, 0)))
    return jnp.concatenate([tp[:, j:j + n_chunks] for j in range(BAND_CHUNKS)], axis=2)


def chunk_band_attention(q, k, v, q_gain, k_gain, rel_bias):
    bsz, L = q.shape[:2]
    n_chunks = L // CHUNK
    qn = _rms32(q, q_gain) * (HEAD_DIM ** -0.5)
    kn = _rms32(k, k_gain)
    qc = qn.reshape(bsz, n_chunks, CHUNK, ATT_HEADS, HEAD_DIM)
    kb = _band(kn, n_chunks)
    vb = _band(v, n_chunks)
    s = jnp.einsum('bcqhd,bckhd->bchqk', qc, kb)
    iq = jnp.arange(CHUNK)
    jk = jnp.arange(BAND)
    dist = LEFT_CHUNKS * CHUNK + iq[:, None] - jk[None, :]
    bias = rel_bias.astype(jnp.float32)[:, jnp.clip(dist, -REL_CLIP, REL_CLIP) + REL_CLIP]
    key_chunk = jnp.arange(n_chunks)[:, None] - LEFT_CHUNKS + (jk // CHUNK)[None, :]
    valid = key_chunk >= 0
    s = jnp.where(valid[None, :, None, None, :], s + bias[None, None], NEG_INF)
    pr = jax.nn.softmax(s, axis=-1).astype(v.dtype)
    o = jnp.einsum('bchqk,bckhd->bcqhd', pr, vb)
    return o.reshape(bsz, L, ATT_WIDTH)


def s5_branch(u, a_re, a_im, log_dt, b_re, b_im, c_re, c_im, d_skip, w_glu, b_glu):
    bsz, L, _ = u.shape
    u32 = u.astype(jnp.float32).reshape(bsz, L, SSM_GROUPS, SSM_GROUP)
    dt = jnp.exp(log_dt.astype(jnp.float32))[:, None]
    ar = a_re.astype(jnp.float32)
    ai = a_im.astype(jnp.float32)
    mag = jnp.exp(dt * ar)
    abar_re = mag * jnp.cos(dt * ai)
    abar_im = mag * jnp.sin(dt * ai)
    den = ar * ar + ai * ai
    nr = abar_re - 1.0
    ni = abar_im
    coef_re = (nr * ar + ni * ai) / den
    coef_im = (ni * ar - nr * ai) / den
    br = b_re.astype(jnp.float32)
    bi = b_im.astype(jnp.float32)
    bbar_re = coef_re[..., None] * br - coef_im[..., None] * bi
    bbar_im = coef_re[..., None] * bi + coef_im[..., None] * br
    bu_re = jnp.einsum('blgc,gpc->blgp', u32, bbar_re)
    bu_im = jnp.einsum('blgc,gpc->blgp', u32, bbar_im)
    at_re = jnp.broadcast_to(abar_re, bu_re.shape)
    at_im = jnp.broadcast_to(abar_im, bu_re.shape)

    def combine(e1, e2):
        a1r, a1i, b1r, b1i = e1
        a2r, a2i, b2r, b2i = e2
        return (a2r * a1r - a2i * a1i,
                a2r * a1i + a2i * a1r,
                a2r * b1r - a2i * b1i + b2r,
                a2r * b1i + a2i * b1r + b2i)

    _, _, h_re, h_im = lax.associative_scan(combine, (at_re, at_im, bu_re, bu_im), axis=1)
    y = jnp.einsum('blgp,gcp->blgc', h_re, c_re.astype(jnp.float32)) - jnp.einsum('blgp,gcp->blgc', h_im, c_im.astype(jnp.float32))
    y = (y + d_skip.astype(jnp.float32).reshape(SSM_GROUPS, SSM_GROUP) * u32).reshape(bsz, L, SSM_WIDTH)
    z = jax.nn.gelu(y).astype(u.dtype) @ w_glu + b_glu
    z_val, z_gate = jnp.split(z, 2, axis=-1)
    return z_val * jax.nn.sigmoid(z_gate)


def hier_moe(xn, w_gr, b_gr, w_er, b_er, w_up, w_gate, w_down):
    gl = (xn @ w_gr).astype(jnp.float32) + b_gr.astype(jnp.float32)
    gsel = jnp.argmax(gl, axis=-1)
    gprob = jnp.take_along_axis(jax.nn.softmax(gl, axis=-1), gsel[..., None], axis=-1)[..., 0]
    el = jnp.einsum('bld,gde->blge', xn, w_er).astype(jnp.float32) + b_er.astype(jnp.float32)
    el = jnp.take_along_axis(el, gsel[..., None, None], axis=2)[:, :, 0]
    top_v, top_i = lax.top_k(el, TOP_K)
    w = jax.nn.softmax(top_v, axis=-1) * gprob[..., None]
    eid = gsel[..., None] * EXPERTS_PER_GROUP + top_i
    gate = jnp.einsum('blk,blke->ble', w, jax.nn.one_hot(eid, N_EXPERTS, dtype=jnp.float32)).astype(xn.dtype)
    out = jnp.zeros_like(xn)
    for g in range(MOE_GROUPS):
        sl = slice(g * EXPERTS_PER_GROUP, (g + 1) * EXPERTS_PER_GROUP)
        hid = jax.nn.silu(jnp.einsum('bld,edf->blef', xn, w_gate[sl])) * jnp.einsum('bld,edf->blef', xn, w_up[sl])
        out = out + jnp.einsum('blef,efd->bld', hid * gate[:, :, sl, None], w_down[sl])
    return out


def setup_inputs(seed: int = 0) -> dict:
    key = jax.random.key(seed)
    ks = iter(jax.random.split(key, 48))
    f32 = jnp.float32

    def nrm(shape, scale):
        return jax.random.normal(next(ks), shape, f32) * scale

    def gain(shape):
        return 1.0 + nrm(shape, 0.02)

    lam_u = jax.random.uniform(next(ks), (DEPTH, LRU_WIDTH), f32, 0.9, 0.999)
    lam_s = lam_u ** (1.0 / LRU_C)
    lru_lambda = jnp.log(lam_s) - jnp.log1p(-lam_s)
    n_idx = jnp.arange(SSM_STATE, dtype=f32)
    ssm_a_im = jnp.broadcast_to(math.pi * n_idx, (DEPTH, SSM_GROUPS, SSM_STATE)) + nrm((DEPTH, SSM_GROUPS, SSM_STATE), 0.01)
    ssm_log_dt = jax.random.uniform(next(ks), (DEPTH, SSM_GROUPS), f32, math.log(1e-3), math.log(1e-1))
    return {
        'x': nrm((BATCH, SEQ, D_MODEL), 1.0),
        'p': nrm((DEPTH, BATCH, SEQ, PLE_DIM), 1.0),
        'mix_gain': gain((DEPTH, D_MODEL)),
        'w_in': nrm((DEPTH, D_MODEL, IN_WIDTH), D_MODEL ** -0.5),
        'conv_w': nrm((DEPTH, CONV_WIDTH, LRU_WIDTH), 0.5),
        'conv_b': nrm((DEPTH, LRU_WIDTH), 0.01),
        'w_rgate': nrm((DEPTH, LRU_BLOCKS, LRU_BLOCK, LRU_BLOCK), LRU_BLOCK ** -0.5),
        'b_rgate': nrm((DEPTH, LRU_BLOCKS, LRU_BLOCK), 0.01),
        'w_igate': nrm((DEPTH, LRU_BLOCKS, LRU_BLOCK, LRU_BLOCK), LRU_BLOCK ** -0.5),
        'b_igate': nrm((DEPTH, LRU_BLOCKS, LRU_BLOCK), 0.01),
        'lru_lambda': lru_lambda,
        'q_gain': gain((DEPTH, HEAD_DIM)),
        'k_gain': gain((DEPTH, HEAD_DIM)),
        'rel_bias': nrm((DEPTH, ATT_HEADS, 2 * REL_CLIP + 1), 0.1),
        'ssm_a_re': -0.5 + nrm((DEPTH, SSM_GROUPS, SSM_STATE), 0.01),
        'ssm_a_im': ssm_a_im,
        'ssm_log_dt': ssm_log_dt,
        'ssm_b_re': nrm((DEPTH, SSM_GROUPS, SSM_STATE, SSM_GROUP), (2 * SSM_GROUP) ** -0.5),
        'ssm_b_im': nrm((DEPTH, SSM_GROUPS, SSM_STATE, SSM_GROUP), (2 * SSM_GROUP) ** -0.5),
        'ssm_c_re': nrm((DEPTH, SSM_GROUPS, SSM_GROUP, SSM_STATE), SSM_STATE ** -0.5),
        'ssm_c_im': nrm((DEPTH, SSM_GROUPS, SSM_GROUP, SSM_STATE), SSM_STATE ** -0.5),
        'ssm_d': nrm((DEPTH, SSM_WIDTH), 1.0),
        'w_glu': nrm((DEPTH, SSM_WIDTH, 2 * SSM_WIDTH), SSM_WIDTH ** -0.5),
        'b_glu': nrm((DEPTH, 2 * SSM_WIDTH), 0.01),
        'w_proj_lru': nrm((DEPTH, LRU_WIDTH, D_MODEL), LRU_WIDTH ** -0.5),
        'w_proj_att': nrm((DEPTH, ATT_WIDTH, D_MODEL), ATT_WIDTH ** -0.5),
        'w_proj_ssm': nrm((DEPTH, SSM_WIDTH, D_MODEL), SSM_WIDTH ** -0.5),
        'w_out': nrm((DEPTH, D_MODEL, D_MODEL), D_MODEL ** -0.5),
        'ffn_gain': gain((DEPTH, D_MODEL)),
        'w_group_router': nrm((DEPTH, D_MODEL, MOE_GROUPS), D_MODEL ** -0.5),
        'b_group_router': nrm((DEPTH, MOE_GROUPS), 0.01),
        'w_expert_router': nrm((DEPTH, MOE_GROUPS, D_MODEL, EXPERTS_PER_GROUP), D_MODEL ** -0.5),
        'b_expert_router': nrm((DEPTH, MOE_GROUPS, EXPERTS_PER_GROUP), 0.01),
        'w_up': nrm((DEPTH, N_EXPERTS, D_MODEL, EXPERT_FF), D_MODEL ** -0.5),
        'w_gate': nrm((DEPTH, N_EXPERTS, D_MODEL, EXPERT_FF), D_MODEL ** -0.5),
        'w_down': nrm((DEPTH, N_EXPERTS, EXPERT_FF, D_MODEL), EXPERT_FF ** -0.5),
        'ple_gain': gain((DEPTH, D_MODEL)),
        'w_ple': nrm((DEPTH, PLE_DIM, D_MODEL), PLE_DIM ** -0.5),
        'w_ple_gate': nrm((DEPTH, D_MODEL, D_MODEL), D_MODEL ** -0.5),
    }


def reference(x, p, mix_gain, w_in, conv_w, conv_b, w_rgate, b_rgate, w_igate, b_igate, lru_lambda,
              q_gain, k_gain, rel_bias, ssm_a_re, ssm_a_im, ssm_log_dt, ssm_b_re, ssm_b_im, ssm_c_re,
              ssm_c_im, ssm_d, w_glu, b_glu, w_proj_lru, w_proj_att, w_proj_ssm, w_out, ffn_gain,
              w_group_router, b_group_router, w_expert_router, b_expert_router, w_up, w_gate, w_down,
              ple_gain, w_ple, w_ple_gate):
    bsz, L, _ = x.shape
    h = x
    for i in range(DEPTH):
        xn = rms_norm(h, mix_gain[i])
        proj = xn @ w_in[i]
        u_lru, q, k, v, u_ssm, gates = jnp.split(proj, SPLIT_POINTS, axis=-1)
        y_lru = rglru_branch(u_lru, conv_w[i], conv_b[i], w_rgate[i], b_rgate[i], w_igate[i], b_igate[i], lru_lambda[i])
        hs = (bsz, L, ATT_HEADS, HEAD_DIM)
        y_att = chunk_band_attention(q.reshape(hs), k.reshape(hs), v.reshape(hs), q_gain[i], k_gain[i], rel_bias[i])
        y_ssm = s5_branch(u_ssm, ssm_a_re[i], ssm_a_im[i], ssm_log_dt[i], ssm_b_re[i], ssm_b_im[i],
                          ssm_c_re[i], ssm_c_im[i], ssm_d[i], w_glu[i], b_glu[i])
        g = jax.nn.sigmoid(gates.astype(jnp.float32)).astype(h.dtype).reshape(bsz, L, N_BRANCHES, D_MODEL)
        merged = (g[:, :, 0] * (y_lru @ w_proj_lru[i])
                  + g[:, :, 1] * (y_att @ w_proj_att[i])
                  + g[:, :, 2] * (y_ssm @ w_proj_ssm[i]))
        h = h + merged @ w_out[i]
        hn = rms_norm(h, ffn_gain[i])
        h = h + hier_moe(hn, w_group_router[i], b_group_router[i], w_expert_router[i], b_expert_router[i],
                         w_up[i], w_gate[i], w_down[i])
        e = p[i] @ w_ple[i]
        pg = jax.nn.sigmoid((rms_norm(h, ple_gain[i]) @ w_ple_gate[i]).astype(jnp.float32)).astype(h.dtype)
        h = h + pg * e
    return h
```

```python
import contextlib
import math
import os as _os
import numpy as np
import concourse.bass as bass
import concourse.mybir as mybir
from concourse.bass_utils import run_bass_kernel_spmd

F32 = mybir.dt.float32
BF16 = mybir.dt.bfloat16
I32 = mybir.dt.int32
ALU = mybir.AluOpType
AF = mybir.ActivationFunctionType
P = 128
NEG = -30000.0
_KOFF = tuple(x for x in _os.environ.get("KOFF", "").split(",") if x)


class Cfg:
    def __init__(self, D=4096, B=2, L=8192, DEPTH=2, T=256):
        self.D, self.B, self.L, self.DEPTH, self.T = D, B, L, DEPTH, T
        self.KT = D // P
        self.LW = D // 2
        self.NLT = self.LW // P
        self.AW = D // 4
        self.NH = self.AW // P
        self.SW = D // 4
        self.NUT = self.SW // P
        self.NST = self.NUT * 4
        self.FF = D // 8
        self.NF = self.FF // P
        self.NE = 32
        self.PLE = 256
        self.INW = self.LW + 3 * self.AW + self.SW + 3 * D
        self.NT = L // T
        self.HALF = 256 if self.FF >= 256 else self.FF
        self.HT = 512 // T
        o = 0
        self.vo = {}
        for name, n in (("mix_gain", self.KT), ("ffn_gain", self.KT), ("ple_gain", self.KT), ("conv_w", self.NLT * 4),
                        ("conv_b", self.NLT), ("lam", self.NLT), ("b_rg", self.NLT), ("b_ig", self.NLT),
                        ("q_gain", 1), ("k_gain", 1), ("ssm_d", self.NUT), ("b_glu", 2 * self.NUT),
                        ("a_re", self.NST), ("a_im", self.NST), ("log_dt", self.NST)):
            self.vo[name] = o
            o += n
        self.NV = o


class _Op:
    __slots__ = ("eng", "fn", "deps", "evs", "sig", "dma", "needs")

    def __init__(self, eng, fn, dma=None):
        self.eng, self.fn, self.dma = eng, fn, dma
        self.deps, self.evs, self.sig, self.needs = [], [], None, False


class Sched:
    ENG = ("sp", "act", "pe", "dve", "pool")

    def __init__(self, nc, stack, n_dma_sems=20):
        self.nc = nc
        self.h = {"sp": nc.sync, "act": nc.scalar, "pe": nc.tensor, "dve": nc.vector, "pool": nc.gpsimd}
        self.sem = {e: stack.enter_context(nc.semaphore("s_" + e)) for e in ("act", "pe", "dve", "pool")}
        self.dsem = [stack.enter_context(nc.semaphore("d%d" % i)) for i in range(n_dma_sems)]
        self.reset_counts()
        self.new_block()
        self.nflush = 0
        self.maxflush = int(_os.environ.get('KMAXFLUSH', '1000000'))
        self.mute = False
        self.nops = 0
        self.maxops = int(_os.environ.get('KMAXOPS', '1000000000'))

    def reset_counts(self):
        self.cnt = {e: 0 for e in self.sem}
        self.dcnt = [0] * len(self.dsem)
        self.dnext = 0
        self.waited = {e: {} for e in self.ENG}

    def new_block(self):
        self.ops = {e: [] for e in self.ENG}
        self.lastw = {}
        self.readers = {}

    def _track(self, op, reads, writes):
        deps = op.deps
        for r in reads:
            w = self.lastw.get(r)
            if w is not None:
                deps.append(w)
            self.readers.setdefault(r, []).append(op)
        for r in writes:
            w = self.lastw.get(r)
            if w is not None:
                deps.append(w)
            for rd in self.readers.get(r, ()):
                if rd is not op:
                    deps.append(rd)
            self.lastw[r] = op
            self.readers[r] = []

    def op(self, eng, fn, reads=(), writes=()):
        o = _Op(eng, fn)
        self.nops += 1
        if self.nops > self.maxops:
            self.mute = True
        if self.mute:
            return o
        self._track(o, reads, writes)
        if eng == "pe":
            o.deps = [d for d in o.deps if d.eng != "pe"]
        elif _os.environ.get("KNOSELF", "") == "1":
            o.deps = [d for d in o.deps if d.eng != eng]
        self.ops[eng].append(o)
        return o

    def dma(self, eng, out, in_, reads=(), writes=()):
        i = self.dnext
        self.dnext = (self.dnext + 1) % len(self.dsem)
        prev = self.dcnt[i]
        self.dcnt[i] += 16
        def _f(e):
            try:
                return e.dma_start(out=out, in_=in_)
            except Exception:
                import traceback; traceback.print_exc(); print("DMA FAIL out", out, "in", in_, flush=True)
                raise
        o = _Op(eng, _f, dma=(self.dsem[i], self.dcnt[i]))
        self.nops += 1
        if self.nops > self.maxops:
            self.mute = True
        if self.mute:
            self.dcnt[i] -= 16
            return o
        if prev:
            o.evs.append((self.dsem[i], prev))
        self._track(o, reads, writes)
        self.ops[eng].append(o)
        return o

    def flush(self, barrier=True):
        nc = self.nc
        self.nflush += 1
        if _os.environ.get('KVERB'):
            print("flush", self.nflush, "nops", self.nops, flush=True)
        if self.nflush >= self.maxflush:
            self.mute = True
        for e in self.ENG:
            for o in self.ops[e]:
                for d in o.deps:
                    d.needs = True
        finals = []
        for e in ("act", "pe", "dve", "pool"):
            if self.ops[e]:
                self.ops[e][-1].needs = True
        for e in ("act", "pe", "dve", "pool"):
            for o in self.ops[e]:
                if o.needs and o.dma is None:
                    self.cnt[e] += 1
                    o.sig = (self.sem[e], self.cnt[e])
            if self.ops[e]:
                last = [o for o in self.ops[e] if o.dma is None]
                if last:
                    finals.append(last[-1].sig)
        dfinal = [(self.dsem[i], self.dcnt[i]) for i in range(len(self.dsem)) if self.dcnt[i]]

        def emit(ename):
            def f(e):
                wd = self.waited[ename]
                for o in self.ops[ename]:
                    evs = list(o.evs)
                    for d in o.deps:
                        evs.append(d.dma if d.dma is not None else d.sig)
                    for (s, v) in evs:
                        k = id(s)
                        if wd.get(k, 0) < v:
                            e.wait_ge(s, v)
                            wd[k] = v
                    ins = o.fn(e)
                    if o.dma is not None:
                        ins.then_inc(o.dma[0], 16)
                    elif o.sig is not None:
                        ins.then_inc(o.sig[0], 1)
                if ename == "sp" and barrier:
                    for (s, v) in finals + dfinal:
                        k = id(s)
                        if wd.get(k, 0) < v:
                            e.wait_ge(s, v)
                            wd[k] = v
            return f

        with nc.Block() as block:
            reg = {"sp": block.sync, "act": block.scalar, "pe": block.tensor, "dve": block.vector, "pool": block.gpsimd}
            for e in self.ENG:
                if self.ops[e] or e == "sp":
                    reg[e](emit(e))
        if barrier:
            nc.all_engine_barrier()
        self.new_block()

    def clear_sems(self):
        nc = self.nc
        sems = list(self.sem.values()) + list(self.dsem)

        with nc.Block() as block:
            @block.sync
            def _(sy):
                for s in sems:
                    sy.sem_clear(s)
        nc.all_engine_barrier()
        self.reset_counts()


class Builder:
    def __init__(self, cfg):
        self.c = cfg
        self.nc = bass.Bass(target_bir_lowering=False)
        self.stack = contextlib.ExitStack()
        self.S = Sched(self.nc, self.stack)
        self.uid = 0
        self.dram = {}

    def din(self, name, shape, dt=F32):
        t = self.nc.dram_tensor(name, list(shape), dt, kind="ExternalInput").ap()
        self.dram[name] = t
        return t

    def dscr(self, name, shape, dt):
        t = self.nc.dram_tensor(name, list(shape), dt).ap()
        self.dram[name] = t
        return t

    def sb(self, stack, name, shape, dt):
        self.uid += 1
        return stack.enter_context(self.nc.sbuf_tensor("%s_%d" % (name, self.uid), list(shape), dt))

    def ps(self, stack, name, shape, dt=F32):
        self.uid += 1
        return stack.enter_context(self.nc.psum_tensor("%s_%d" % (name, self.uid), list(shape), dt))

    def act(self, out, in_, func, reads, writes, bias=None, scale=None, eng="act"):
        kw = {}
        if bias is not None:
            kw["bias"] = bias
        if scale is not None:
            kw["scale"] = scale
        return self.S.op("act", lambda e: e.activation(out=out, in_=in_, func=func, **kw), reads, writes)

    def ts(self, eng, out, in0, s1, s2, op0, op1, reads, writes):
        if op1 is None:
            return self.S.op(eng, lambda e: e.tensor_scalar(out=out, in0=in0, scalar1=s1, scalar2=None, op0=op0), reads, writes)
        return self.S.op(eng, lambda e: e.tensor_scalar(out=out, in0=in0, scalar1=s1, scalar2=s2, op0=op0, op1=op1), reads, writes)

    def tt(self, eng, out, in0, in1, op, reads, writes):
        return self.S.op(eng, lambda e: e.tensor_tensor(out=out, in0=in0, in1=in1, op=op), reads, writes)

    def stt(self, eng, out, in0, scalar, in1, op0, op1, reads, writes):
        return self.S.op(eng, lambda e: e.scalar_tensor_tensor(out=out, in0=in0, scalar=scalar, in1=in1, op0=op0, op1=op1), reads, writes)

    def cp(self, eng, out, in_, reads, writes):
        if eng == "act":
            return self.S.op("act", lambda e: e.activation(out=out, in_=in_, func=AF.Copy), reads, writes)
        return self.S.op(eng, lambda e: e.tensor_copy(out=out, in_=in_), reads, writes)

    def mm(self, out, lhsT, rhs, start, stop, reads, writes):
        return self.S.op("pe", lambda e: e.matmul(out, lhsT=lhsT, rhs=rhs, start=start, stop=stop), reads, writes)

    def declare(self):
        c = self.c
        d = self.din
        d("xT", [P, c.B * c.NT * c.KT * c.T])
        [d("pT%d" % l_, [c.PLE, c.B * c.L]) for l_ in range(c.DEPTH)]
        hw = c.HALF
        d("w_in", [c.DEPTH, c.INW // 256, P, c.KT, 256])
        d("w_proj_lru", [c.DEPTH, c.D // 256, P, c.NLT, 256])
        d("w_proj_att", [c.DEPTH, c.D // 256, P, c.NH, 256])
        d("w_proj_ssm", [c.DEPTH, c.D // 256, P, c.NUT, 256])
        d("w_out", [c.DEPTH, c.D // 256, P, c.KT, 256])
        d("w_glu", [c.DEPTH, 2 * c.SW // 256, P, c.NUT, 256])
        d("w_up", [c.DEPTH, c.NE, c.FF // hw, P, c.KT, hw])
        d("w_gate", [c.DEPTH, c.NE, c.FF // hw, P, c.KT, hw])
        d("w_down", [c.DEPTH, 4, c.D // 256, P, 8 * c.NF, 256])
        d("w_ple", [c.DEPTH, c.PLE, c.D])
        d("w_ple_gate", [c.DEPTH, c.D // 256, P, c.KT, 256])
        d("w_rgate", [c.DEPTH, c.NLT, P, P])
        d("w_igate", [c.DEPTH, c.NLT, P, P])
        d("vecs", [c.DEPTH, P, c.NV])
        d("routerW", [c.DEPTH, P, c.KT, 36])
        d("routerB", [c.DEPTH, P, 36])
        d("bT_re", [c.DEPTH, P, c.NST, P])
        d("bT_im", [c.DEPTH, P, c.NST, P])
        d("cT_re", [c.DEPTH, P, c.NST, P])
        d("cT_im", [c.DEPTH, P, c.NST, P])
        d("att_bias", [c.DEPTH, P, c.NH, 640])
        d("att_mask", [P, 640])
        d("ident", [P, P])
        d("iota1", [P, c.T])
        self.outT = self.nc.dram_tensor("outT", [P, c.B * c.NT * c.KT * c.T], F32, kind="ExternalOutput").ap()
        s = self.dscr
        for n in ("w_in", "w_proj_lru", "w_proj_att", "w_proj_ssm", "w_out", "w_glu", "w_up", "w_gate", "w_down",
                  "w_ple", "w_ple_gate", "w_rgate", "w_igate"):
            for l_ in range(c.DEPTH):
                s(n + "_b%d" % l_, list(self.dram[n].shape)[1:], BF16)
        for b_ in range(c.B):
            s("hs%d" % b_, [P, c.NT * c.KT * c.T], F32)
        s("tab_cos", [c.DEPTH, P, c.NST, c.T], F32)
        s("tab_sin", [c.DEPTH, P, c.NST, c.T], F32)
        s("bbT_re", [c.DEPTH, P, c.NST, P], BF16)
        s("bbT_im", [c.DEPTH, P, c.NST, P], BF16)
        s("btab", [c.DEPTH, P, c.NH, 640], F32)
        s("lvec", [c.DEPTH, P, 8 * c.NST + 2 * c.NLT], F32)

    def convert_weights(self):
        S = self.S
        for n in ("w_in", "w_proj_lru", "w_proj_att", "w_proj_ssm", "w_out", "w_glu", "w_up", "w_gate", "w_down",
                  "w_ple", "w_ple_gate", "w_rgate", "w_igate"):
          for l_ in range(self.c.DEPTH):
            src = self.dram[n][l_]
            dst = self.dram[n + "_b%d" % l_]
            sh = src.shape
            tot = int(np.prod(sh))
            cols = 4096
            assert tot % cols == 0
            rows = tot // cols
            names = " ".join("a%d" % i for i in range(len(sh)))
            s2 = src.rearrange("%s -> (%s)" % (names, names)).rearrange("(r c) -> r c", c=cols)
            d2 = dst.rearrange("%s -> (%s)" % (names, names)).rearrange("(r c) -> r c", c=cols)
            step = 256
            for r0 in range(0, rows, step):
                r1 = min(rows, r0 + step)
                S.dma("pool", d2[r0:r1, :], s2[r0:r1, :], reads=(), writes=())
        S.flush()

    def layer_setup(self, l):
        c, S, nc = self.c, self.S, self.nc
        vo = c.vo
        NST, T = c.NST, c.T
        with contextlib.ExitStack() as st:
            vec = self.sb(st, "vec", [P, c.NV], F32)
            idn = self.sb(st, "idn", [P, P], F32)
            io1 = self.sb(st, "io1", [P, T], F32)
            lv = self.sb(st, "lv", [P, 8 * NST + 2 * c.NLT], F32)
            tmp = self.sb(st, "tmp", [P, 16, NST], F32)
            ti = self.sb(st, "ti", [P, NST], I32)
            S.dma("sp", vec[:], self.dram["vecs"][l], writes=["vec"])
            S.dma("sp", idn[:], self.dram["ident"], writes=["idn"])
            S.dma("sp", io1[:], self.dram["iota1"], writes=["io1"])
            are = vec[:, vo["a_re"]:vo["a_re"] + NST]
            aim = vec[:, vo["a_im"]:vo["a_im"] + NST]
            ldt = vec[:, vo["log_dt"]:vo["log_dt"] + NST]
            k = [0]

            def T_(i):
                return tmp[:, i, :]
            dt_, dar, th, mag = T_(0), T_(1), T_(2), lv[:, 0:NST]
            rd = ["vec"]
            self.act(dt_, ldt, AF.Exp, rd, ["t0"])
            self.tt("dve", dar, dt_, are, ALU.mult, ["t0", "vec"], ["t1"])
            self.tt("dve", th, dt_, aim, ALU.mult, ["t0", "vec"], ["t2"])
            self.act(mag, dar, AF.Exp, ["t1"], ["lv0"])

            def reduce_angle(dst, src, srckey, dstkey, shape_i, tkeys):
                kf, kf2 = tkeys
                C1 = 6.28125
                C2 = 2.0 * math.pi - C1
                self.ts("dve", kf[0], src, 1.0 / (2.0 * math.pi), None, ALU.mult, None, [srckey], [kf[1]])
                self.cp("dve", shape_i[0], kf[0], [kf[1]], [shape_i[1]])
                self.cp("dve", kf[0], shape_i[0], [shape_i[1]], [kf[1]])
                self.stt("dve", kf2[0], kf[0], -C1, src, ALU.mult, ALU.add, [kf[1], srckey], [kf2[1]])
                self.stt("dve", dst, kf[0], -C2, kf2[0], ALU.mult, ALU.add, [kf[1], kf2[1]], [dstkey])

            thr = lv[:, NST:2 * NST]
            reduce_angle(thr, th, "t2", "lv1", (ti[:], "ti"), ((T_(3), "t3"), (T_(4), "t4")))
            sn, cs, thc = T_(5), T_(6), T_(7)
            self.act(sn, thr, AF.Sin, ["lv1"], ["t5"])
            self.ts("dve", T_(8), thr, math.pi / 2, None, ALU.add, None, ["lv1"], ["t8"])
            reduce_angle(thc, T_(8), "t8", "t7", (ti[:], "ti"), ((T_(3), "t3"), (T_(4), "t4")))
            self.act(cs, thc, AF.Sin, ["t7"], ["t6"])
            nr, ni, den = T_(9), T_(10), T_(11)
            self.tt("dve", nr, mag, cs, ALU.mult, ["lv0", "t6"], ["t9"])
            self.ts("dve", nr, nr, -1.0, None, ALU.add, None, ["t9"], ["t9"])
            self.tt("dve", ni, mag, sn, ALU.mult, ["lv0", "t5"], ["t10"])
            self.tt("dve", den, are, are, ALU.mult, ["vec"], ["t11"])
            self.tt("dve", T_(12), aim, aim, ALU.mult, ["vec"], ["t12"])
            self.tt("dve", den, den, T_(12), ALU.add, ["t11", "t12"], ["t11"])
            self.S.op("dve", lambda e: e.reciprocal(out=den, in_=den), ["t11"], ["t11"])
            cre = lv[:, 2 * NST:3 * NST]
            cim = lv[:, 3 * NST:4 * NST]
            self.tt("dve", T_(12), nr, are, ALU.mult, ["t9", "vec"], ["t12"])
            self.tt("dve", T_(13), ni, aim, ALU.mult, ["t10", "vec"], ["t13"])
            self.tt("dve", T_(12), T_(12), T_(13), ALU.add, ["t12", "t13"], ["t12"])
            self.tt("dve", cre, T_(12), den, ALU.mult, ["t12", "t11"], ["lv2"])
            self.tt("dve", T_(14), ni, are, ALU.mult, ["t10", "vec"], ["t14"])
            self.tt("dve", T_(15), nr, aim, ALU.mult, ["t9", "vec"], ["t15"])
            self.tt("dve", T_(14), T_(14), T_(15), ALU.subtract, ["t14", "t15"], ["t14"])
            self.tt("dve", cim, T_(14), den, ALU.mult, ["t14", "t11"], ["lv3"])
            ncim = lv[:, 4 * NST:5 * NST]
            self.ts("dve", ncim, cim, -1.0, None, ALU.mult, None, ["lv3"], ["lv4"])
            lam = vec[:, vo["lam"]:vo["lam"] + c.NLT]
            o5 = 8 * NST
            cst = lv[:, o5:o5 + c.NLT]
            cst2 = lv[:, o5 + c.NLT:o5 + 2 * c.NLT]
            tl = self.sb(st, "tl", [P, c.NLT], F32)
            self.act(tl[:], lam, AF.Exp, ["vec"], ["tl"], scale=-1.0)
            self.act(tl[:], tl[:], AF.Ln, ["tl"], ["tl"], bias=1.0)
            self.ts("dve", cst, tl[:], -8.0, None, ALU.mult, None, ["tl"], ["lv5"])
            self.ts("dve", cst2, tl[:], -16.0, None, ALU.mult, None, ["tl"], ["lv6"])
            S.dma("sp", self.dram["lvec"][l], lv[:], reads=["lv0", "lv1", "lv2", "lv3", "lv4", "lv5", "lv6"], writes=["lvd"])
            ang = self.sb(st, "ang", [P, 2, T], F32)
            k1 = self.sb(st, "k1", [P, 2, T], F32)
            k2 = self.sb(st, "k2", [P, 2, T], F32)
            ki = self.sb(st, "ki", [P, 2, T], I32)
            tc = self.sb(st, "tc", [P, 2, NST, T], F32)
            for s_ in range(NST):
                b = s_ % 2
                a_ = ang[:, b, :]
                self.ts("dve", a_, io1[:], thr[:, s_:s_ + 1], None, ALU.mult, None, ["io1", "lv1"], [("ang", b)])
                red = k2[:, b, :]
                reduce_angle(red, a_, ("ang", b), ("red", b), (ki[:, b, :], ("ki", b)), ((k1[:, b, :], ("k1", b)), (k2[:, b, :], ("k2", b))))
                self.act(tc[:, 1, s_, :], red, AF.Sin, [("red", b), ("k2", b)], [("tc", 1, s_)])
                self.ts("dve", a_, red, math.pi / 2, None, ALU.add, None, [("red", b), ("k2", b)], [("ang", b)])
                reduce_angle(red, a_, ("ang", b), ("red", b), (ki[:, b, :], ("ki", b)), ((k1[:, b, :], ("k1", b)), (k2[:, b, :], ("k2", b))))
                self.act(tc[:, 0, s_, :], red, AF.Sin, [("red", b), ("k2", b)], [("tc", 0, s_)])
            S.dma("sp", self.dram["tab_cos"][l], tc[:, 0], reads=[("tc", 0, s_) for s_ in range(NST)], writes=["tcd"])
            S.dma("sp", self.dram["tab_sin"][l], tc[:, 1], reads=[("tc", 1, s_) for s_ in range(NST)], writes=["tsd"])
            S.flush()
        with contextlib.ExitStack() as st:
            lv = self.sb(st, "lv", [P, 8 * NST + 2 * c.NLT], F32)
            idn = self.sb(st, "idn", [P, P], F32)
            bre = self.sb(st, "bre", [P, NST, P], F32)
            bim = self.sb(st, "bim", [P, NST, P], F32)
            w1 = self.sb(st, "w1", [P, 2, P], BF16)
            w2 = self.sb(st, "w2", [P, 2, P], BF16)
            w1f = self.sb(st, "w1f", [P, 2, P], F32)
            w2f = self.sb(st, "w2f", [P, 2, P], F32)
            idb = self.sb(st, "idb", [P, P], BF16)
            ob = self.sb(st, "ob", [P, 2, NST, P], BF16)
            pt2 = [self.ps(st, "pt%d" % i, [P, 512], F32) for i in range(2)]
            S.dma("sp", lv[:], self.dram["lvec"][l], writes=["lv"])
            S.dma("sp", idn[:], self.dram["ident"], writes=["idn"])
            S.dma("sp", bre[:], self.dram["bT_re"][l], writes=["bre"])
            S.dma("sp", bim[:], self.dram["bT_im"][l], writes=["bim"])
            cre = lv[:, 2 * NST:3 * NST]
            cim = lv[:, 3 * NST:4 * NST]
            ncim = lv[:, 4 * NST:5 * NST]
            self.cp("dve", idb[:], idn[:], ["idn"], ["idn"])
            for s_ in range(NST):
                b = s_ % 2
                self.ts("dve", w1f[:, b, :], bre[:, s_, :], cre[:, s_:s_ + 1], None, ALU.mult, None, ["lv", "bre"], [("w1f", b)])
                self.stt("dve", w1[:, b, :], bim[:, s_, :], ncim[:, s_:s_ + 1], w1f[:, b, :], ALU.mult, ALU.add, ["lv", "bim", ("w1f", b)], [("w1", b)])
                self.ts("dve", w2f[:, b, :], bim[:, s_, :], cre[:, s_:s_ + 1], None, ALU.mult, None, ["lv", "bim"], [("w2f", b)])
                self.stt("dve", w2[:, b, :], bre[:, s_, :], cim[:, s_:s_ + 1], w2f[:, b, :], ALU.mult, ALU.add, ["lv", "bre", ("w2f", b)], [("w2", b)])
                if _os.environ.get("KSKIP", "") == "notr":
                    self.cp("act", ob[:, 0, s_, :], w1[:, b, :], [("w1", b)], [("ob", 0, s_)])
                    self.cp("act", ob[:, 1, s_, :], w2[:, b, :], [("w2", b)], [("ob", 1, s_)])
                    continue
                self.mm(pt2[b][:, 0:P], w1[:, b, :], idb[:], True, True, [("w1", b), "idn"], [("pt", b)])
                self.mm(pt2[b][:, P:2 * P], w2[:, b, :], idb[:], True, True, [("w2", b), "idn"], [("pt", b)])
                if _os.environ.get("KSKIP", "") == "nocopy":
                    self.cp("act", ob[:, 0, s_, :], w1[:, b, :], [("pt", b)], [("ob", 0, s_)])
                    self.cp("act", ob[:, 1, s_, :], w2[:, b, :], [("pt", b)], [("ob", 1, s_)])
                    continue
                if _os.environ.get("KSKIP", "") == "dvecopy":
                    self.cp("dve", ob[:, 0, s_, :], pt2[b][:, 0:P], [("pt", b)], [("ob", 0, s_)])
                    self.cp("dve", ob[:, 1, s_, :], pt2[b][:, P:2 * P], [("pt", b)], [("ob", 1, s_)])
                    continue
                self.cp("act", ob[:, 0, s_, :], pt2[b][:, 0:P], [("pt", b)], [("ob", 0, s_)])
                self.cp("act", ob[:, 1, s_, :], pt2[b][:, P:2 * P], [("pt", b)], [("ob", 1, s_)])
            S.dma("sp", self.dram["bbT_re"][l], ob[:, 0], reads=[("ob", 0, s_) for s_ in range(NST)], writes=["o1"])
            S.dma("sp", self.dram["bbT_im"][l], ob[:, 1], reads=[("ob", 1, s_) for s_ in range(NST)], writes=["o2"])
            S.flush()
        with contextlib.ExitStack() as st:
            ab = self.sb(st, "ab", [P, c.NH, 640], F32)
            am = self.sb(st, "am", [P, 640], F32)
            S.dma("sp", ab[:], self.dram["att_bias"][l], writes=["ab"])
            S.dma("sp", am[:], self.dram["att_mask"], writes=["am"])
            for h in range(c.NH):
                self.tt("dve", ab[:, h, :], ab[:, h, :], am[:], ALU.add, ["ab", "am"], [("ab", h)])
            S.dma("sp", self.dram["btab"][l], ab[:], reads=["ab"] + [("ab", h) for h in range(c.NH)], writes=["btd"])
            S.flush()

    def wload(self, slot, skey, src2d, kt, col0, ncols, eng="sp"):
        view = slot[:, 0:kt * ncols].rearrange("p (k n) -> p k n", k=kt)
        self.S.dma(eng, view, src2d[col0 // ncols], reads=(), writes=[skey])
        return view

    def rmsnorm(self, st, h, hkey, gain, gkey, out_bf, okey, ones, extra=None):
        c, S = self.c, self.S
        T, KT = c.T, c.KT
        sq = self.sb(st, "sq", [P, 2, T], BF16)
        rstd = self.sb(st, "rstd", [P, T], F32)
        pss_t = self.ps(st, "pss", [P, 512], F32)
        pss = pss_t[:, 0:T]
        for kt in range(KT):
            b = kt % 2
            self.act(sq[:, b, :], h[:, kt, :], AF.Square, [(hkey, kt)], [("sq", b)])
            self.mm(pss, ones[:], sq[:, b, :], kt == 0, kt == KT - 1, ["ones", ("sq", b)], ["pss"])
        self.act(rstd[:], pss, AF.Sqrt, ["pss"], ["rstd"], bias=self.eps_col[:, 0:1])
        S.op("dve", lambda e: e.reciprocal(out=rstd[:], in_=rstd[:]), ["rstd"], ["rstd"])
        hn = self.sb(st, "hnf", [P, 2, T], F32) if extra is not None else None
        hl = self.sb(st, "hnl", [P, 2, T], BF16) if extra is not None else None
        for kt in range(KT):
            if extra is None:
                self.stt("dve", out_bf[:, kt, :], h[:, kt, :], gain[:, kt:kt + 1], rstd[:], ALU.mult, ALU.mult,
                         [(hkey, kt), gkey, "rstd"], [(okey, kt)])
            else:
                b = kt % 2
                self.stt("dve", hn[:, b, :], h[:, kt, :], gain[:, kt:kt + 1], rstd[:], ALU.mult, ALU.mult,
                         [(hkey, kt), gkey, "rstd"], [("hnf", b)])
                self.cp("act", out_bf[:, kt, :], hn[:, b, :], [("hnf", b)], [(okey, kt)])
                self.tt("dve", hl[:, b, :], hn[:, b, :], out_bf[:, kt, :], ALU.subtract, [("hnf", b), (okey, kt)], [("hnl", b)])
                extra(kt, out_bf[:, kt, :], (okey, kt), hl[:, b, :], ("hnl", b))

    def tile_body(self, l, b, it, st0, pers):
        c, S, nc = self.c, self.S, self.nc
        T, KT, D = c.T, c.KT, c.D
        vo = c.vo
        vec, lv, ones, idn, ones_b = pers["vec"], pers["lv"], pers["ones"], pers["idn"], pers["ones_b"]
        KTh, Vh, lstate, sstate, uhalo, hbias = pers["KTh"], pers["Vh"], pers["lstate"], pers["sstate"], pers["uhalo"], pers["hbias"]
        wrg, wig = pers["wrg"], pers["wig"]
        bsl = slice(b * c.L, (b + 1) * c.L)
        hsz = c.NT * c.KT * c.T
        src_h = self.dram["xT"][:, b * hsz:(b + 1) * hsz] if l == 0 else self.dram["hs%d" % b]
        dst_h = self.outT[:, b * hsz:(b + 1) * hsz] if l == c.DEPTH - 1 else self.dram["hs%d" % b]
        htile = bass.ts(it, c.KT * c.T)
        W = lambda n: self.dram[n + "_b%d" % l]
        tsl = bass.ts(it, T)
        NW = 3
        NPS = 4

        with contextlib.ExitStack() as st_it:
            xn = self.sb(st_it, "xn", [P, KT, T], BF16)
            yb = self.sb(st_it, "yb", [P, KT, T], BF16)
            wbuf = [self.sb(st_it, "wb%d" % i, [P, 8192], BF16) for i in range(NW)]
            psb = [self.ps(st_it, "psb%d" % i, [P, 512], F32) for i in range(NPS)]
            wi = [0]
            pi = [0]

            def wslot():
                i = wi[0] % NW
                wi[0] += 1
                return wbuf[i], ("wb", i)

            def pslot():
                i = pi[0] % NPS
                pi[0] += 1
                return psb[i][:, 0:T], ("ps", i)

            st_ussm = contextlib.ExitStack()
            st_qn = contextlib.ExitStack()
            st_ulru = contextlib.ExitStack()
            ussm = self.sb(st_ussm, "ussm", [P, c.NUT, T], F32)
            ussb = self.sb(st_ussm, "ussb", [P, c.NUT, T], BF16)
            qn = self.sb(st_qn, "qn", [P, c.NH, T], BF16)
            ulru = self.sb(st_ulru, "ulru", [P, c.NLT, T + 3], F32)
            with contextlib.ExitStack() as st:
                h = self.sb(st, "h", [P, KT, T], F32)
                S.dma("sp", h[:].rearrange("p k t -> p (k t)"), src_h[:, htile], writes=[("h", k) for k in range(KT)])
                gain = vec[:, vo["mix_gain"]:vo["mix_gain"] + KT]
                self.rmsnorm(st, h, "h", gain, "vec", xn, "xn", ones)
                S.flush()
            with contextlib.ExitStack() as st:
                qk = self.sb(st, "qk", [P, 2, T], F32)
                sq2 = self.sb(st, "sq2", [P, T], BF16)
                rs = self.sb(st, "rs", [P, T], F32)
                self.cp("pool", ulru[:, :, 0:3], uhalo[:], ["uhalo"], [("ulru_h",)])
                nblk = (c.LW + 3 * c.AW + c.SW) // 256
                qgs = pers["qgs"]
                kg = vec[:, vo["k_gain"]:vo["k_gain"] + 1]
                nq = 0
                for blk in range(nblk):
                    col0 = blk * 256
                    slot, skey = wslot()
                    wv = self.wload(slot, skey, W("w_in"), KT, col0, 256)
                    seg = "lru" if col0 < c.LW else ("q" if col0 < c.LW + c.AW else ("k" if col0 < c.LW + 2 * c.AW else ("v" if col0 < c.LW + 3 * c.AW else "ssm")))
                    if seg == "v":
                        cv0 = col0 - (c.LW + 2 * c.AW)
                        for tsb in range(T // P):
                            pso, pkey = pslot()
                            for kt in range(KT):
                                self.mm(pso[:, 0:256], xn[:, kt, tsb * P:(tsb + 1) * P], wv[:, kt, :], kt == 0, kt == KT - 1, [("xn", kt), skey], [pkey])
                            self.cp("act", Vh[:, c.HT * (T // P) + tsb, cv0:cv0 + 256], pso[:, 0:256], [pkey], [("Vh", tsb, cv0)])
                        continue
                    for j in range(2):
                        pso, pkey = pslot()
                        for kt in range(KT):
                            self.mm(pso, wv[:, kt, j * P:(j + 1) * P], xn[:, kt, :], kt == 0, kt == KT - 1, [skey, ("xn", kt)], [pkey])
                        fo = (col0 + j * P)
                        if seg == "lru":
                            m = fo // P
                            self.cp("act", ulru[:, m, 3:3 + T], pso, [pkey], [("ulru", m)])
                        elif seg == "ssm":
                            m = (fo - (c.LW + 3 * c.AW)) // P
                            self.cp("act", ussm[:, m, :], pso, [pkey], [("ussm", m)])
                            self.cp("dve", ussb[:, m, :], ussm[:, m, :], [("ussm", m)], [("ussb", m)])
                        else:
                            hh = ((fo - c.LW) % c.AW) // P
                            bq = nq % 2
                            nq += 1
                            self.cp("act", qk[:, bq, :], pso, [pkey], [("qk", bq)])
                            self.act(sq2[:], qk[:, bq, :], AF.Square, [("qk", bq)], ["sq2"])
                            ps2, pkey2 = pslot()
                            self.mm(ps2, pers["ones_h"][:], sq2[:], True, True, ["sq2"], [pkey2])
                            self.act(rs[:], ps2, AF.Sqrt, [pkey2], ["rs"], bias=self.eps_col[:, 0:1])
                            S.op("dve", lambda e: e.reciprocal(out=rs[:], in_=rs[:]), ["rs"], ["rs"])
                            if seg == "q":
                                self.stt("dve", qn[:, hh, :], qk[:, bq, :], qgs[:, 0:1], rs[:], ALU.mult, ALU.mult, [("qk", bq), "rs"], [("qn", hh)])
                            else:
                                self.stt("dve", KTh[:, hh, c.HT * T:(c.HT + 1) * T], qk[:, bq, :], kg, rs[:], ALU.mult, ALU.mult, [("qk", bq), "rs"], [("KTh", "cur", hh)])
                S.flush()
            self.cp("pool", uhalo[:], ulru[:, :, T:T + 3], [], ["uhalo"])

            with contextlib.ExitStack() as s2:
                cw = vec[:, vo["conv_w"]:vo["conv_w"] + 4 * c.NLT]
                cb = vec[:, vo["conv_b"]:vo["conv_b"] + c.NLT]
                brg = vec[:, vo["b_rg"]:vo["b_rg"] + c.NLT]
                big = vec[:, vo["b_ig"]:vo["b_ig"] + c.NLT]
                o5 = 8 * c.NST
                cst = lv[:, o5:o5 + c.NLT]
                cst2 = lv[:, o5 + c.NLT:o5 + 2 * c.NLT]
                NB = 2
                xc = self.sb(s2, "xc", [P, NB, T], F32)
                xcb = self.sb(s2, "xcb", [P, NB, T], BF16)
                rr = self.sb(s2, "rr", [P, NB, T], F32)
                gg = self.sb(s2, "gg", [P, NB, T], F32)
                aa = self.sb(s2, "aa", [P, NB, T], F32)
                a2 = self.sb(s2, "a2", [P, NB, T], F32)
                hh_ = self.sb(s2, "hh", [P, NB, T], F32)
                for m in range(c.NLT):
                    q = m % NB
                    X = xc[:, q, :]
                    self.ts("dve", X, ulru[:, m, 0:T], cw[:, 4 * m:4 * m + 1], cb[:, m:m + 1], ALU.mult, ALU.add, [], [("xc", q)])
                    for j in range(1, 4):
                        self.stt("dve", X, ulru[:, m, j:j + T], cw[:, 4 * m + j:4 * m + j + 1], X, ALU.mult, ALU.add, [("xc", q)], [("xc", q)])
                    self.cp("act", xcb[:, q, :], X, [("xc", q)], [("xcb", q)])
                    p1, k1 = pslot()
                    self.mm(p1, wrg[:, m, :], xcb[:, q, :], True, True, [("xcb", q)], [k1])
                    p2, k2 = pslot()
                    self.mm(p2, wig[:, m, :], xcb[:, q, :], True, True, [("xcb", q)], [k2])
                    self.act(rr[:, q, :], p1, AF.Sigmoid, [k1], [("rr", q)], bias=brg[:, m:m + 1])
                    self.act(gg[:, q, :], p2, AF.Sigmoid, [k2], [("gg", q)], bias=big[:, m:m + 1])
                    self.act(aa[:, q, :], rr[:, q, :], AF.Exp, [("rr", q)], [("aa", q)], scale=cst[:, m:m + 1])
                    self.act(a2[:, q, :], rr[:, q, :], AF.Exp, [("rr", q)], [("a2", q)], scale=cst2[:, m:m + 1])
                    self.ts("dve", a2[:, q, :], a2[:, q, :], -1.0, 1.0, ALU.mult, ALU.add, [("a2", q)], [("a2", q)])
                    self.act(a2[:, q, :], a2[:, q, :], AF.Sqrt, [("a2", q)], [("a2", q)])
                    self.tt("dve", gg[:, q, :], gg[:, q, :], X, ALU.mult, [("gg", q), ("xc", q)], [("gg", q)])
                    self.tt("dve", gg[:, q, :], gg[:, q, :], a2[:, q, :], ALU.mult, [("gg", q), ("a2", q)], [("gg", q)])
                    S.op("dve", lambda e, q=q, m=m: e.tensor_tensor_scan(out=hh_[:, q, :], data0=aa[:, q, :], data1=gg[:, q, :], initial=lstate[:, m:m + 1], op0=ALU.mult, op1=ALU.add),
                         [("aa", q), ("gg", q), ("ls", m)], [("hh", q)])
                    self.cp("dve", lstate[:, m:m + 1], hh_[:, q, T - 1:T], [("hh", q)], [("ls", m)])
                    self.cp("act", yb[:, m, :], hh_[:, q, :], [("hh", q)], [("yb", m)])
                S.flush()
            st_ulru.close()

            with contextlib.ExitStack() as s2:
                bt = self.sb(s2, "bt", [P, c.NH, 640], F32)
                S.dma("sp", bt[:], self.dram["btab"][l], writes=["bt"])
                ssb = self.sb(s2, "ssb", [P, 2, 640], F32)
                ptb = self.sb(s2, "ptb", [P, 2, 640], BF16)
                rc = self.sb(s2, "rc", [P, 2, P], F32)
                psS = [self.ps(s2, "psS%d" % i, [P, 512], F32) for i in range(2)]
                psO2 = [self.ps(s2, "psO%d" % i, [P, 512], F32) for i in range(2)]
                NG = T // P
                NHB = c.HT * T // P
                n = 0
                for hh in range(c.NH):
                    for g in range(NG):
                        q = n % 2
                        n += 1
                        psOv, psOs = psO2[q][:, 0:P], psO2[q][:, P:2 * P]
                        for kb in range(5):
                            kc = P * (g + kb)
                            dst = psS[0][:, kb * P:(kb + 1) * P] if kb < 4 else psS[1][:, 0:P]
                            self.mm(dst, KTh[:, hh, kc:kc + P], qn[:, hh, g * P:(g + 1) * P], True, True, [], [("psS", 0 if kb < 4 else 1)])
                        self.tt("dve", ssb[:, q, 0:512], psS[0][:], bt[:, hh, 0:512], ALU.add, [("psS", 0), "bt"], [("ssb", q, 0)])
                        self.tt("dve", ssb[:, q, 512:640], psS[1][:, 0:P], bt[:, hh, 512:640], ALU.add, [("psS", 1), "bt"], [("ssb", q, 1)])
                        kb = 0
                        while kb < 5 and g + kb < NHB:
                            jb = g + kb
                            self.act(ptb[:, q, kb * P:(kb + 1) * P], ssb[:, q, kb * P:(kb + 1) * P], AF.Exp, [("ssb", q, 0), ("ssb", q, 1), "hb"], [("ptb", q, kb)], bias=hbias[:, jb:jb + 1])
                            kb += 1
                        if kb < 5:
                            self.act(ptb[:, q, kb * P:640], ssb[:, q, kb * P:640], AF.Exp, [("ssb", q, 0), ("ssb", q, 1)], [("ptb", q, k) for k in range(kb, 5)])
                        for kb in range(5):
                            vt = g + kb
                            self.mm(psOv, Vh[:, vt, hh * P:(hh + 1) * P], ptb[:, q, kb * P:(kb + 1) * P], kb == 0, kb == 4, [("ptb", q, kb)], [("psO", q)])
                        for kb in range(5):
                            self.mm(psOs, ones_b[:], ptb[:, q, kb * P:(kb + 1) * P], kb == 0, kb == 4, [("ptb", q, kb)], [("psO", q)])
                        S.op("dve", lambda e, q=q, psOs=psOs: e.reciprocal(out=rc[:, q, :], in_=psOs), [("psO", q)], [("rc", q)])
                        self.tt("dve", yb[:, c.NLT + hh, g * P:(g + 1) * P], psOv, rc[:, q, :], ALU.mult, [("psO", q), ("rc", q)], [("yba", hh, g)])
                S.flush()
            st_qn.close()
            with contextlib.ExitStack() as s2:
                nvt = T // P
                kt_tmp = self.sb(s2, "kttmp", [P, c.NH, c.HT * T], BF16)
                self.cp("pool", kt_tmp[:], KTh[:, :, T:(c.HT + 1) * T], [], ["kttmp"])
                self.cp("pool", KTh[:, :, 0:c.HT * T], kt_tmp[:], ["kttmp"], ["KTh"])
                v_tmp = self.sb(s2, "vtmp", [P, c.HT * nvt, c.AW], BF16)
                self.cp("dve", v_tmp[:], Vh[:, nvt:(c.HT + 1) * nvt, :], [], ["vtmp"])
                self.cp("dve", Vh[:, 0:c.HT * nvt, :], v_tmp[:], ["vtmp"], ["Vh"])
                hb_tmp = self.sb(s2, "hbtmp", [P, NHB], F32)
                S.op("dve", lambda e: e.memset(hb_tmp[:], 0.0), [], ["hbt0"])
                if NHB > nvt:
                    self.cp("dve", hb_tmp[:, 0:NHB - nvt], hbias[:, nvt:NHB], ["hbt0"], ["hbt"])
                self.cp("dve", hbias[:, 0:NHB], hb_tmp[:], ["hbt", "hbt0"], ["hb"])
                S.flush()

            with contextlib.ExitStack() as s2:
                NST = c.NST
                bbr = self.sb(s2, "bbr", [P, 2, 4, P], BF16)
                bbi = self.sb(s2, "bbi", [P, 2, 4, P], BF16)
                ccr = self.sb(s2, "ccr", [P, 2, 4, P], F32)
                cci = self.sb(s2, "cci", [P, 2, 4, P], F32)
                NB = 2
                tcs = self.sb(s2, "tcs", [P, NB, 2, T], F32)
                dec = self.sb(s2, "dec", [P, NB, T], F32)
                t1 = self.sb(s2, "t1", [P, NB, T], F32)
                t2 = self.sb(s2, "t2", [P, NB, T], F32)
                bre_ = self.sb(s2, "bre_", [P, NB, T], F32)
                bim_ = self.sb(s2, "bim_", [P, NB, T], F32)
                gre = self.sb(s2, "gre", [P, NB, T], F32)
                gim = self.sb(s2, "gim", [P, NB, T], F32)
                hre = self.sb(s2, "hre", [P, 4, T], BF16)
                him = self.sb(s2, "him", [P, 4, T], BF16)
                ccrb = self.sb(s2, "ccrb", [P, 2, 4, P], BF16)
                ccib = self.sb(s2, "ccib", [P, 2, 4, P], BF16)
                ysb = self.sb(s2, "ysb", [P, 2, T], F32)
                gl = self.sb(s2, "gl", [P, c.NUT, T], BF16)
                zero = pers["zero"]
                mag = lv[:, 0:NST]
                dsk = vec[:, vo["ssm_d"]:vo["ssm_d"] + c.NUT]
                for ut in range(c.NUT):
                    ub = ut % 2
                    S.dma("sp", bbr[:, ub], self.dram["bbT_re"][l][:, ut * 4:ut * 4 + 4, :], writes=[("bbr", ub)])
                    S.dma("sp", bbi[:, ub], self.dram["bbT_im"][l][:, ut * 4:ut * 4 + 4, :], writes=[("bbi", ub)])
                    S.dma("sp", ccr[:, ub], self.dram["cT_re"][l][:, ut * 4:ut * 4 + 4, :], writes=[("ccr", ub)])
                    S.dma("sp", cci[:, ub], self.dram["cT_im"][l][:, ut * 4:ut * 4 + 4, :], writes=[("cci0", ub)])
                    self.ts("pool", ccib[:, ub], cci[:, ub], -1.0, None, ALU.mult, None, [("cci0", ub)], [("cci", ub)])
                    self.cp("pool", ccrb[:, ub], ccr[:, ub], [("ccr", ub)], [("ccrb", ub)])
                    for s4 in range(4):
                        s_ = ut * 4 + s4
                        q = s_ % NB
                        S.dma("sp", tcs[:, q, 0, :], self.dram["tab_cos"][l][:, s_, :], writes=[("tcs", q, 0)])
                        S.dma("sp", tcs[:, q, 1, :], self.dram["tab_sin"][l][:, s_, :], writes=[("tcs", q, 1)])
                        CO, SI = tcs[:, q, 0, :], tcs[:, q, 1, :]
                        kC, kS = ("tcs", q, 0), ("tcs", q, 1)
                        p1, k1 = pslot()
                        self.mm(p1, bbr[:, ub, s4, :], ussb[:, ut, :], True, True, [("bbr", ub)], [k1])
                        p2, k2 = pslot()
                        self.mm(p2, bbi[:, ub, s4, :], ussb[:, ut, :], True, True, [("bbi", ub)], [k2])
                        self.tt("dve", t1[:, q, :], p1, CO, ALU.mult, [k1, kC], [("t1", q)])
                        self.tt("dve", t2[:, q, :], p2, SI, ALU.mult, [k2, kS], [("t2", q)])
                        self.tt("pool", bre_[:, q, :], t1[:, q, :], t2[:, q, :], ALU.add, [("t1", q), ("t2", q)], [("bre_", q)])
                        self.tt("dve", t1[:, q, :], p2, CO, ALU.mult, [k2, kC], [("t1", q)])
                        self.tt("dve", t2[:, q, :], p1, SI, ALU.mult, [k1, kS], [("t2", q)])
                        self.tt("pool", bim_[:, q, :], t1[:, q, :], t2[:, q, :], ALU.subtract, [("t1", q), ("t2", q)], [("bim_", q)])
                        self.ts("pool", dec[:, q, :], zero[:], mag[:, s_:s_ + 1], None, ALU.add, None, [], [("dec", q)])
                        S.op("dve", lambda e, q=q, s_=s_: e.tensor_tensor_scan(out=gre[:, q, :], data0=dec[:, q, :], data1=bre_[:, q, :], initial=sstate[:, 0, s_:s_ + 1], op0=ALU.mult, op1=ALU.add),
                             [("dec", q), ("bre_", q), ("ss", 0, s_)], [("gre", q)])
                        S.op("dve", lambda e, q=q, s_=s_: e.tensor_tensor_scan(out=gim[:, q, :], data0=dec[:, q, :], data1=bim_[:, q, :], initial=sstate[:, 1, s_:s_ + 1], op0=ALU.mult, op1=ALU.add),
                             [("dec", q), ("bim_", q), ("ss", 1, s_)], [("gim", q)])
                        self.tt("pool", t1[:, q, :], gre[:, q, :], CO, ALU.mult, [("gre", q), kC], [("t1", q)])
                        self.tt("pool", t2[:, q, :], gim[:, q, :], SI, ALU.mult, [("gim", q), kS], [("t2", q)])
                        self.tt("pool", hre[:, s4, :], t1[:, q, :], t2[:, q, :], ALU.subtract, [("t1", q), ("t2", q)], [("hre", s4)])
                        self.tt("pool", sstate[:, 0, s_:s_ + 1], t1[:, q, T - 1:T], t2[:, q, T - 1:T], ALU.subtract, [("t1", q), ("t2", q), ("gre", q), ("gim", q)], [("ss", 0, s_)])
                        self.tt("dve", t1[:, q, :], gre[:, q, :], SI, ALU.mult, [("gre", q), kS], [("t1", q)])
                        self.tt("dve", t2[:, q, :], gim[:, q, :], CO, ALU.mult, [("gim", q), kC], [("t2", q)])
                        self.tt("dve", him[:, s4, :], t1[:, q, :], t2[:, q, :], ALU.add, [("t1", q), ("t2", q)], [("him", s4)])
                        self.tt("dve", sstate[:, 1, s_:s_ + 1], t1[:, q, T - 1:T], t2[:, q, T - 1:T], ALU.add, [("t1", q), ("t2", q), ("gre", q), ("gim", q)], [("ss", 1, s_)])
                    py, ky = pslot()
                    for s4 in range(4):
                        self.mm(py, ccrb[:, ub, s4, :], hre[:, s4, :], s4 == 0, False, [("ccrb", ub), ("hre", s4)], [ky])
                        self.mm(py, ccib[:, ub, s4, :], him[:, s4, :], False, s4 == 3, [("cci", ub), ("him", s4)], [ky])
                    qy = ut % 2
                    Y = ysb[:, qy, :]
                    self.stt("dve", Y, ussm[:, ut, :], dsk[:, ut:ut + 1], py, ALU.mult, ALU.add, [ky], [("ysb", qy)])
                    G1 = t1[:, qy, :]
                    G2 = t2[:, qy, :]
                    self.tt("dve", G1, Y, Y, ALU.mult, [("ysb", qy)], [("t1", qy)])
                    self.ts("dve", G1, G1, 0.044715, 1.0, ALU.mult, ALU.add, [("t1", qy)], [("t1", qy)])
                    self.tt("dve", G1, G1, Y, ALU.mult, [("t1", qy), ("ysb", qy)], [("t1", qy)])
                    self.act(G2, G1, AF.Tanh, [("t1", qy)], [("t2", qy)], scale=0.7978845608028654)
                    self.ts("dve", G2, G2, 1.0, 0.5, ALU.add, ALU.mult, [("t2", qy)], [("t2", qy)])
                    self.tt("dve", gl[:, ut, :], G2, Y, ALU.mult, [("t2", qy), ("ysb", qy)], [("gl", ut)])
                bgl = vec[:, vo["b_glu"]:vo["b_glu"] + 2 * c.NUT]
                sg = self.sb(s2, "sg", [P, 2, T], F32)
                for j2 in range(c.SW // 256):
                    slot, skey = wslot()
                    wv = self.wload(slot, skey, W("w_glu"), c.NUT, j2 * 256, 256)
                    slot2, skey2 = wslot()
                    wg = self.wload(slot2, skey2, W("w_glu"), c.NUT, c.SW + j2 * 256, 256)
                    for j in range(2):
                        m = j2 * 2 + j
                        pv, kv = pslot()
                        for kt in range(c.NUT):
                            self.mm(pv, wv[:, kt, j * P:(j + 1) * P], gl[:, kt, :], kt == 0, kt == c.NUT - 1, [skey, ("gl", kt)], [kv])
                        pg, kg_ = pslot()
                        for kt in range(c.NUT):
                            self.mm(pg, wg[:, kt, j * P:(j + 1) * P], gl[:, kt, :], kt == 0, kt == c.NUT - 1, [skey2, ("gl", kt)], [kg_])
                        self.act(sg[:, j, :], pg, AF.Sigmoid, [kg_], [("sg", j)], bias=bgl[:, c.NUT + m:c.NUT + m + 1])
                        self.stt("dve", yb[:, c.NLT + c.NH + m, :], pv, bgl[:, m:m + 1], sg[:, j, :], ALU.add, ALU.mult, [kv, ("sg", j)], [("yb", c.NLT + c.NH + m)])
                S.flush()
            st_ussm.close()

            with contextlib.ExitStack() as st:
                h = self.sb(st, "h3", [P, KT, T], F32)
                S.dma("sp", h[:].rearrange("p k t -> p (k t)"), src_h[:, htile], writes=[("h", k) for k in range(KT)])
                sil = self.sb(st, "sil", [P, 2, T], F32)
                with contextlib.ExitStack() as s3:
                    mg = self.sb(s3, "mg", [P, KT, T], BF16)
                    sgm = self.sb(s3, "sgm", [P, 2, T], F32)
                    mm_ = self.sb(s3, "mm_", [P, 3, 2, T], F32)
                    gbase = c.LW + 3 * c.AW + c.SW
                    pinfo = [("w_proj_lru", 0, c.NLT), ("w_proj_att", c.NLT, c.NH), ("w_proj_ssm", c.NLT + c.NH, c.NUT)]
                    for m2 in range(D // 256):
                        for br in range(3):
                            slot, skey = wslot()
                            wv = self.wload(slot, skey, W("w_in"), KT, gbase + br * D + m2 * 256, 256)
                            pname, y0, nk = pinfo[br]
                            slot2, skey2 = wslot()
                            vv = self.wload(slot2, skey2, W(pname), nk, m2 * 256, 256)
                            for j in range(2):
                                pg, kg_ = pslot()
                                for kt in range(KT):
                                    self.mm(pg, wv[:, kt, j * P:(j + 1) * P], xn[:, kt, :], kt == 0, kt == KT - 1, [skey, ("xn", kt)], [kg_])
                                self.act(sgm[:, j, :], pg, AF.Sigmoid, [kg_], [("sgm", j)])
                                pp, kp = pslot()
                                for kt in range(nk):
                                    self.mm(pp, vv[:, kt, j * P:(j + 1) * P], yb[:, y0 + kt, :], kt == 0, kt == nk - 1, [skey2], [kp])
                                self.tt("dve", mm_[:, br, j, :], sgm[:, j, :], pp, ALU.mult, [("sgm", j), kp], [("mm_", br, j)])
                                if ("lru", "att", "ssm")[br] in _KOFF:
                                    S.op("dve", lambda e, br=br, j=j: e.memset(mm_[:, br, j, :], 0.0), [], [("mm_", br, j)])
                        for j in range(2):
                            m = m2 * 2 + j
                            self.tt("pool", mm_[:, 0, j, :], mm_[:, 0, j, :], mm_[:, 1, j, :], ALU.add, [("mm_", 0, j), ("mm_", 1, j)], [("mm_", 0, j)])
                            self.tt("pool", mg[:, m, :], mm_[:, 0, j, :], mm_[:, 2, j, :], ALU.add, [("mm_", 0, j), ("mm_", 2, j)], [("mg", m)])
                    for m2 in range(D // 256):
                        slot, skey = wslot()
                        wv = self.wload(slot, skey, W("w_out"), KT, m2 * 256, 256)
                        for j in range(2):
                            m = m2 * 2 + j
                            po, ko = pslot()
                            for kt in range(KT):
                                self.mm(po, wv[:, kt, j * P:(j + 1) * P], mg[:, kt, :], kt == 0, kt == KT - 1, [skey, ("mg", kt)], [ko])
                            self.tt("dve", h[:, m, :], h[:, m, :], po, ALU.add, [("h", m), ko], [("h", m)])
                    S.flush()
                NS = T // P
                psr2 = [self.ps(st, "psr%d" % i, [P, 512], F32) for i in range(NS)]

                rwh, rwl = pers["rwh"], pers["rwl"]

                def router_hook(kt, hi, khi, lo, klo):
                    for sbt in range(NS):
                        dst = psr2[sbt][:, 0:36]
                        sl = slice(sbt * P, (sbt + 1) * P)
                        self.mm(dst, hi[:, sl], rwh[:, kt, :], kt == 0, False, [khi], ["psr"])
                        self.mm(dst, lo[:, sl], rwh[:, kt, :], False, False, [klo], ["psr"])
                        self.mm(dst, hi[:, sl], rwl[:, kt, :], False, kt == KT - 1, [khi], ["psr"])
                gain = vec[:, vo["ffn_gain"]:vo["ffn_gain"] + KT]
                with contextlib.ExitStack() as s3:
                    self.rmsnorm(s3, h, "h", gain, "vec", xn, "xn", ones, extra=router_hook)
                    S.flush()
                gT = self.sb(st, "gT", [32, T], F32)
                gTh = self.sb(st, "gTh", [32, T], BF16)
                gTl = self.sb(st, "gTl", [32, T], BF16)
                with contextlib.ExitStack() as s3:
                    lg = self.sb(s3, "lg", [P, 36], F32)
                    w8 = self.sb(s3, "w8", [P, 8, 8], F32)
                    g32 = self.sb(s3, "g32", [P, 32], F32)
                    g32h = self.sb(s3, "g32h", [P, 32], BF16)
                    g32l = self.sb(s3, "g32l", [P, 32], BF16)
                    idb = pers["idb"]
                    c1 = self.sb(s3, "c1", [P, 8], F32)
                    pT_t = self.ps(s3, "pT_", [P, 512], F32)
                    pT_ = pT_t[0:32, 0:P]
                    rb = pers["rb"]
                    AX = mybir.AxisListType.X
                    for sbt in range(NS):
                        k = lambda x: ("r", x)
                        self.tt("dve", lg[:], psr2[sbt][:, 0:36], rb[:], ALU.add, ["psr"], [k("lg")])
                        gmax, gsum, mk4, e4 = c1[:, 0:1], c1[:, 1:2], w8[:, 0, 0:4], w8[:, 1, 0:4]
                        S.op("dve", lambda e, gmax=gmax: e.tensor_reduce(out=gmax, in_=lg[:, 0:4], axis=AX, op=ALU.max), [k("lg")], [k("gmax")])
                        self.ts("dve", mk4, lg[:, 0:4], gmax, None, ALU.is_equal, None, [k("lg"), k("gmax")], [k("mk4")])
                        self.ts("dve", e4, lg[:, 0:4], gmax, None, ALU.subtract, None, [k("lg"), k("gmax")], [k("e4")])
                        self.act(e4, e4, AF.Exp, [k("e4")], [k("e4")])
                        S.op("dve", lambda e, gsum=gsum, e4=e4: e.tensor_reduce(out=gsum, in_=e4, axis=AX, op=ALU.add), [k("e4")], [k("gsum")])
                        S.op("dve", lambda e, gsum=gsum: e.reciprocal(out=gsum, in_=gsum), [k("gsum")], [k("gsum")])
                        els = w8[:, 2, :]
                        self.ts("dve", els, lg[:, 4:12], mk4[:, 0:1], None, ALU.mult, None, [k("lg"), k("mk4")], [k("els")])
                        for g in range(1, 4):
                            self.stt("dve", els, lg[:, 4 + 8 * g:12 + 8 * g], mk4[:, g:g + 1], els, ALU.mult, ALU.add, [k("lg"), k("mk4"), k("els")], [k("els")])
                        m1, m2v, dd = c1[:, 2:3], c1[:, 3:4], c1[:, 4:5]
                        S.op("dve", lambda e, m1=m1, els=els: e.tensor_reduce(out=m1, in_=els, axis=AX, op=ALU.max), [k("els")], [k("m1")])
                        k1_, el2, k2_ = w8[:, 3, :], w8[:, 4, :], w8[:, 5, :]
                        self.ts("dve", k1_, els, m1, None, ALU.is_equal, None, [k("els"), k("m1")], [k("k1")])
                        self.stt("dve", el2, k1_, NEG, els, ALU.mult, ALU.add, [k("k1"), k("els")], [k("el2")])
                        S.op("dve", lambda e, m2v=m2v, el2=el2: e.tensor_reduce(out=m2v, in_=el2, axis=AX, op=ALU.max), [k("el2")], [k("m2")])
                        self.ts("dve", k2_, el2, m2v, None, ALU.is_equal, None, [k("el2"), k("m2")], [k("k2")])
                        self.tt("dve", dd, m1, m2v, ALU.subtract, [k("m1"), k("m2")], [k("dd")])
                        w1c, w2c = c1[:, 5:6], c1[:, 6:7]
                        self.act(w1c, dd, AF.Sigmoid, [k("dd")], [k("w1")])
                        self.ts("dve", w2c, w1c, -1.0, 1.0, ALU.mult, ALU.add, [k("w1")], [k("w2")])
                        self.tt("dve", w1c, w1c, gsum, ALU.mult, [k("w1"), k("gsum")], [k("w1")])
                        self.tt("dve", w2c, w2c, gsum, ALU.mult, [k("w2"), k("gsum")], [k("w2")])
                        gw = w8[:, 6, :]
                        self.ts("dve", gw, k1_, w1c, None, ALU.mult, None, [k("k1"), k("w1")], [k("gw")])
                        self.stt("dve", gw, k2_, w2c, gw, ALU.mult, ALU.add, [k("k2"), k("w2"), k("gw")], [k("gw")])
                        for g in range(4):
                            self.ts("dve", g32[:, 8 * g:8 * g + 8], gw, mk4[:, g:g + 1], None, ALU.mult, None, [k("gw"), k("mk4")], [k(("g32", g))])
                        g32keys = [k(("g32", g)) for g in range(4)]
                        self.cp("dve", g32h[:], g32[:], g32keys, [k("g32h")])
                        self.tt("dve", g32l[:], g32[:], g32h[:], ALU.subtract, g32keys + [k("g32h")], [k("g32l")])
                        self.mm(pT_, g32h[:], idb[:], True, False, [k("g32h")], [k("pT")])
                        self.mm(pT_, g32l[:], idb[:], False, True, [k("g32l")], [k("pT")])
                        self.cp("act", gT[:, sbt * P:(sbt + 1) * P], pT_, [k("pT")], [("gT", sbt)])
                    gTk = [("gT", sbt) for sbt in range(NS)]
                    self.cp("dve", gTh[:], gT[:], gTk, ["gTh"])
                    self.tt("dve", gTl[:], gT[:], gTh[:], ALU.subtract, gTk + ["gTh"], ["gTl"])
                    S.flush()
                with contextlib.ExitStack() as s3:
                    sel = pers["sel"]
                    NF = c.NF
                    hid = self.sb(s3, "hid", [P, 8 * NF, T], BF16)
                    psG2 = [self.ps(s3, "psG%d" % i, [P, 512], F32) for i in range(2)]
                    half = 256 if c.FF >= 256 else c.FF
                    nhalf = c.FF // half
                    ftile_per_half = half // P
                    for grp in range(4):
                        for ei in range(8):
                            ex = grp * 8 + ei
                            qg = ex % 2
                            pG, kG = psG2[qg][:, 0:T], ("psG", qg)
                            self.mm(pG, sel[:, ex, :], gTh[:], True, False, ["gTh"], [kG])
                            self.mm(pG, sel[:, ex, :], gTl[:], False, True, ["gTl"], [kG])
                            for hf in range(nhalf):
                                s1, sk1 = wslot()
                                wg = self.wload(s1, sk1, W("w_gate")[ex], KT, hf * half, half)
                                s2_, sk2 = wslot()
                                wu = self.wload(s2_, sk2, W("w_up")[ex], KT, hf * half, half)
                                for j in range(ftile_per_half):
                                    f = hf * ftile_per_half + j
                                    pg, kg_ = pslot()
                                    for kt in range(KT):
                                        self.mm(pg, wg[:, kt, j * P:(j + 1) * P], xn[:, kt, :], kt == 0, kt == KT - 1, [sk1], [kg_])
                                    pu, ku = pslot()
                                    for kt in range(KT):
                                        self.mm(pu, wu[:, kt, j * P:(j + 1) * P], xn[:, kt, :], kt == 0, kt == KT - 1, [sk2], [ku])
                                    qs = f % 2
                                    self.act(sil[:, qs, :], pg, AF.Silu, [kg_], [("sil", qs)])
                                    self.tt("dve", sil[:, qs, :], sil[:, qs, :], pu, ALU.mult, [("sil", qs), ku], [("sil", qs)])
                                    self.tt("dve", hid[:, ei * NF + f, :], sil[:, qs, :], pG, ALU.mult, [("sil", qs), kG], [("hid", ei * NF + f)])
                        wd = W("w_down")[grp]
                        for m2 in range(D // 256):
                            slot, skey = wslot()
                            view = slot[:, 0:8 * NF * 256].rearrange("p (k n) -> p k n", k=8 * NF)
                            S.dma("sp", view, wd[m2], writes=[skey])
                            for j in range(2):
                                m = m2 * 2 + j
                                po, ko = pslot()
                                for kf in range(8 * NF):
                                    self.mm(po, view[:, kf, j * P:(j + 1) * P], hid[:, kf, :], kf == 0, kf == 8 * NF - 1, [skey, ("hid", kf)], [ko])
                                if "moe" not in _KOFF:
                                    self.tt("dve", h[:, m, :], h[:, m, :], po, ALU.add, [("h", m), ko], [("h", m)])
                                else:
                                    self.cp("dve", sil[:, 0, :], po, [ko], [("sil", 0)])
                    S.flush()
                gain = vec[:, vo["ple_gain"]:vo["ple_gain"] + KT]
                with contextlib.ExitStack() as s3:
                    self.rmsnorm(s3, h, "h", gain, "vec", xn, "xn", ones)
                    S.flush()
                pf = self.sb(st, "pf", [P, 2, T], F32)
                pb = self.sb(st, "pb", [P, 2, T], BF16)
                S.dma("sp", pf[:], self.dram["pT%d" % l][:, bsl].rearrange("(k p) t -> p k t", p=P)[:, :, tsl], writes=["pf"])
                self.cp("dve", pb[:], pf[:], ["pf"], ["pb"])
                wpl = self.sb(st, "wpl", [P, 2, 2, 256], BF16)
                for m2 in range(D // 256):
                    slot, skey = wslot()
                    wv = self.wload(slot, skey, W("w_ple_gate"), KT, m2 * 256, 256)
                    q = m2 % 2
                    S.dma("sp", wpl[:, q], W("w_ple").rearrange("(k p) n -> p k n", p=P)[:, :, m2 * 256:(m2 + 1) * 256], writes=[("wpl", q)])
                    for j in range(2):
                        m = m2 * 2 + j
                        pg, kg_ = pslot()
                        for kt in range(KT):
                            self.mm(pg, wv[:, kt, j * P:(j + 1) * P], xn[:, kt, :], kt == 0, kt == KT - 1, [skey], [kg_])
                        pe_, ke = pslot()
                        for kt in range(2):
                            self.mm(pe_, wpl[:, q, kt, j * P:(j + 1) * P], pb[:, kt, :], kt == 0, kt == 1, [("wpl", q), "pb"], [ke])
                        self.act(sil[:, j, :], pg, AF.Sigmoid, [kg_], [("sil", j)])
                        self.tt("dve", sil[:, j, :], sil[:, j, :], pe_, ALU.mult, [("sil", j), ke], [("sil", j)])
                        if "ple" not in _KOFF:
                            self.tt("pool", h[:, m, :], h[:, m, :], sil[:, j, :], ALU.add, [("h", m), ("sil", j)], [("h", m)])
                S.dma("sp", dst_h[:, htile], h[:].rearrange("p k t -> p (k t)"), reads=[("h", k) for k in range(KT)], writes=[("hd", 0)])
                S.flush()

    def build(self):
        c, S, nc = self.c, self.S, self.nc
        self.declare()
        with self.stack:
            self.eps_col = self.sb(self.stack, "eps", [P, 1], F32)
            S.op("dve", lambda e: e.memset(self.eps_col[:], 1e-6), [], ["eps"])
            S.flush()
            self.convert_weights()
            for l in range(c.DEPTH):
                self.layer_setup(l)
                with contextlib.ExitStack() as st:
                    pers = {}
                    vo = c.vo
                    pers["vec"] = vec = self.sb(st, "vecp", [P, c.NV], F32)
                    pers["lv"] = lv = self.sb(st, "lvp", [P, 8 * c.NST + 2 * c.NLT], F32)
                    pers["ones"] = ones = self.sb(st, "ones", [P, P], BF16)
                    pers["ones_h"] = ones_h = self.sb(st, "ones_h", [P, P], BF16)
                    pers["ones_b"] = ones_b = self.sb(st, "ones_b", [P, P], BF16)
                    ones32 = self.sb(st, "ones32", [32, P], F32)
                    pers["sel"] = sel = self.sb(st, "sel", [32, 32, P], BF16)
                    pers["idn"] = idn = self.sb(st, "idnp", [P, P], F32)
                    pers["idb"] = idb = self.sb(st, "idbp", [P, P], BF16)
                    pers["rwh"] = rwh = self.sb(st, "rwh", [P, c.KT, 36], BF16)
                    pers["rwl"] = rwl = self.sb(st, "rwl", [P, c.KT, 36], BF16)
                    pers["zero"] = zero = self.sb(st, "zero", [P, c.T], F32)
                    pers["qgs"] = qgs = self.sb(st, "qgs", [P, 1], F32)
                    rw = self.sb(st, "rw", [P, c.KT, 36], F32)
                    pers["rb"] = rb = self.sb(st, "rb", [P, 36], F32)
                    pers["wrg"] = wrg = self.sb(st, "wrg", [P, c.NLT, P], BF16)
                    pers["wig"] = wig = self.sb(st, "wig", [P, c.NLT, P], BF16)
                    pers["KTh"] = KTh = self.sb(st, "KTh", [P, c.NH, (c.HT + 1) * c.T], BF16)
                    pers["Vh"] = Vh = self.sb(st, "Vh", [P, (c.HT + 1) * (c.T // P), c.AW], BF16)
                    pers["lstate"] = lstate = self.sb(st, "lstate", [P, c.NLT], F32)
                    pers["sstate"] = sstate = self.sb(st, "sstate", [P, 2, c.NST], F32)
                    pers["uhalo"] = uhalo = self.sb(st, "uhalo", [P, c.NLT, 3], F32)
                    pers["hbias"] = hbias = self.sb(st, "hbias", [P, max(2, c.HT * c.T // P)], F32)
                    S.dma("sp", vec[:], self.dram["vecs"][l], writes=["v"])
                    S.dma("sp", lv[:], self.dram["lvec"][l], writes=["lv"])
                    S.dma("sp", idn[:], self.dram["ident"], writes=["idn"])
                    S.dma("sp", rw[:], self.dram["routerW"][l], writes=["rw"])
                    S.dma("sp", rb[:], self.dram["routerB"][l], writes=["rb"])
                    S.dma("sp", wrg[:], self.dram["w_rgate_b%d" % l].rearrange("h i j -> i h j"), writes=["wrg"])
                    S.dma("sp", wig[:], self.dram["w_igate_b%d" % l].rearrange("h i j -> i h j"), writes=["wig"])
                    S.op("dve", lambda e: e.memset(ones[:], 1.0 / c.D), [], ["o1"])
                    S.op("dve", lambda e: e.memset(ones_h[:], 1.0 / P), [], ["o2"])
                    S.op("dve", lambda e: e.memset(ones_b[:], 1.0), [], ["o3"])
                    S.op("dve", lambda e: e.memset(ones32[:], 1.0), [], ["o4"])
                    S.op("dve", lambda e: e.memset(zero[:], 0.0), [], ["o5"])
                    self.ts("dve", qgs[:], vec[:, vo["q_gain"]:vo["q_gain"] + 1], float(P) ** -0.5, None, ALU.mult, None, ["v"], ["qgs"])
                    self.cp("dve", idb[:], idn[:], ["idn"], ["idb"])
                    self.cp("dve", rwh[:], rw[:], ["rw"], ["rwh"])
                    self.tt("dve", rwl[:], rw[:], rwh[:], ALU.subtract, ["rw", "rwh"], ["rwl"])
                    for ex in range(32):
                        self.ts("dve", sel[:, ex, :], ones32[:], idn[0:32, ex:ex + 1], None, ALU.mult, None, ["o4", "idn"], [("sel", ex)])
                    S.flush()
                    for b in range(c.B):
                        S.op("dve", lambda e: e.memset(KTh[:], 0.0), [], ["a1"])
                        S.op("dve", lambda e: e.memset(Vh[:], 0.0), [], ["a2"])
                        S.op("dve", lambda e: e.memset(lstate[:], 0.0), [], ["a3"])
                        S.op("dve", lambda e: e.memset(sstate[:], 0.0), [], ["a4"])
                        S.op("dve", lambda e: e.memset(uhalo[:], 0.0), [], ["a5"])
                        S.op("dve", lambda e: e.memset(hbias[:], NEG), [], ["a6"])
                        S.flush()
                        S.clear_sems()
                        with nc.Fori(0, c.NT) as it:
                            self.tile_body(l, b, it, st, pers)
                            S.clear_sems()
        return nc


def _pack_inputs(c, inp):
    f = lambda a: np.ascontiguousarray(np.asarray(a, dtype=np.float32))
    D = c.D
    m = {}
    nb = np.asarray(inp["x"]).shape[0]
    x6 = np.asarray(inp["x"], np.float32).reshape(nb, c.NT, c.T, c.KT, P)
    m["xT"] = f(np.transpose(x6, (4, 0, 1, 3, 2)).reshape(P, nb * c.NT * c.KT * c.T))
    pT_all = np.transpose(inp["p"], (0, 3, 1, 2)).reshape(c.DEPTH, c.PLE, nb * c.L)
    for l_ in range(c.DEPTH):
        m["pT%d" % l_] = f(pT_all[l_])
    def blk(w, bw):
        w = np.asarray(w, np.float32)
        dd, K_, N_ = w.shape
        return f(w.reshape(dd, K_ // P, P, N_ // bw, bw).transpose(0, 3, 2, 1, 4))
    for n in ("w_in", "w_proj_lru", "w_proj_att", "w_proj_ssm", "w_out", "w_glu", "w_ple_gate"):
        m[n] = blk(inp[n], 256)
    m["w_ple"] = f(inp["w_ple"])
    hw = c.HALF
    for n in ("w_up", "w_gate"):
        w = np.asarray(inp[n], np.float32).reshape(c.DEPTH, c.NE, c.KT, P, c.FF // hw, hw)
        m[n] = f(w.transpose(0, 1, 4, 3, 2, 5))
    w = np.asarray(inp["w_down"], np.float32).reshape(c.DEPTH, 4, 8, c.NF, P, c.D // 256, 256)
    m["w_down"] = f(w.transpose(0, 1, 5, 4, 2, 3, 6).reshape(c.DEPTH, 4, c.D // 256, P, 8 * c.NF, 256))
    m["w_rgate"] = f(inp["w_rgate"])
    m["w_igate"] = f(inp["w_igate"])
    vecs = np.zeros((c.DEPTH, P, c.NV), np.float32)

    def put(name, arr):
        a = np.asarray(arr, np.float32).reshape(c.DEPTH, -1, P).transpose(0, 2, 1)
        vecs[:, :, c.vo[name]:c.vo[name] + a.shape[2]] = a
    put("mix_gain", inp["mix_gain"])
    put("ffn_gain", inp["ffn_gain"])
    put("ple_gain", inp["ple_gain"])
    cw = np.asarray(inp["conv_w"], np.float32)
    cw = cw.reshape(c.DEPTH, 4, c.NLT, P).transpose(0, 3, 2, 1).reshape(c.DEPTH, P, c.NLT * 4)
    vecs[:, :, c.vo["conv_w"]:c.vo["conv_w"] + c.NLT * 4] = cw
    put("conv_b", inp["conv_b"])
    put("lam", inp["lru_lambda"])
    put("b_rg", np.asarray(inp["b_rgate"]).reshape(c.DEPTH, -1))
    put("b_ig", np.asarray(inp["b_igate"]).reshape(c.DEPTH, -1))
    put("q_gain", inp["q_gain"])
    put("k_gain", inp["k_gain"])
    put("ssm_d", inp["ssm_d"])
    put("b_glu", inp["b_glu"])
    put("a_re", np.asarray(inp["ssm_a_re"]).reshape(c.DEPTH, -1))
    put("a_im", np.asarray(inp["ssm_a_im"]).reshape(c.DEPTH, -1))
    G = c.SW // 16
    put("log_dt", np.repeat(np.asarray(inp["ssm_log_dt"], np.float32)[:, :, None], 64, axis=2).reshape(c.DEPTH, -1))
    m["vecs"] = vecs
    rw = np.concatenate([np.asarray(inp["w_group_router"], np.float32),
                         np.transpose(np.asarray(inp["w_expert_router"], np.float32), (0, 2, 1, 3)).reshape(c.DEPTH, D, 32)], axis=2)
    m["routerW"] = f(rw.reshape(c.DEPTH, c.KT, P, 36).transpose(0, 2, 1, 3))
    rbias = np.concatenate([np.asarray(inp["b_group_router"], np.float32), np.asarray(inp["b_expert_router"], np.float32).reshape(c.DEPTH, 32)], axis=1)
    m["routerB"] = f(np.broadcast_to(rbias[:, None, :], (c.DEPTH, P, 36)))
    bre = np.asarray(inp["ssm_b_re"], np.float32)
    bim = np.asarray(inp["ssm_b_im"], np.float32)
    cre = np.asarray(inp["ssm_c_re"], np.float32)
    cim = np.asarray(inp["ssm_c_im"], np.float32)
    bT_re = np.zeros((c.DEPTH, P, c.NST, P), np.float32)
    bT_im = np.zeros_like(bT_re)
    cT_re = np.zeros_like(bT_re)
    cT_im = np.zeros_like(bT_re)
    for g in range(G):
        st_, two = g // 2, g % 2
        ch0 = (g % 8) * 16
        bT_re[:, two * 64:(two + 1) * 64, st_, ch0:ch0 + 16] = bre[:, g]
        bT_im[:, two * 64:(two + 1) * 64, st_, ch0:ch0 + 16] = bim[:, g]
        cT_re[:, two * 64:(two + 1) * 64, st_, ch0:ch0 + 16] = np.transpose(cre[:, g], (0, 2, 1))
        cT_im[:, two * 64:(two + 1) * 64, st_, ch0:ch0 + 16] = np.transpose(cim[:, g], (0, 2, 1))
    m["bT_re"], m["bT_im"], m["cT_re"], m["cT_im"] = bT_re, bT_im, cT_re, cT_im
    j = np.arange(P)[:, None, None]
    kb = np.arange(5)[None, :, None]
    i = np.arange(P)[None, None, :]
    dist = 512 - 128 * kb + i - j
    idx = np.clip(dist, -128, 128) + 128
    rbv = np.asarray(inp["rel_bias"], np.float32)
    ab = rbv[:, :, idx]
    m["att_bias"] = f(np.transpose(ab, (0, 2, 1, 3, 4)).reshape(c.DEPTH, P, c.NH, 640))
    kchunk = 2 * kb + j // 64
    qc = i // 64
    allowed = (kchunk >= qc) & (kchunk <= 8 + qc)
    m["att_mask"] = f(np.where(allowed, 0.0, NEG).astype(np.float32).reshape(P, 640))
    m["ident"] = np.eye(P, dtype=np.float32)
    m["iota1"] = f(np.broadcast_to(np.arange(1, c.T + 1, dtype=np.float32)[None, :], (P, c.T)))
    return m


_CACHE = {}


def run_cfg(c, inputs, trace=False):
    key = (c.D, c.B, c.L, c.DEPTH, c.T)
    if key not in _CACHE:
        _CACHE[key] = Builder(c).build()
    nc = _CACHE[key]
    m = _pack_inputs(c, inputs)
    nb = np.asarray(inputs["x"]).shape[0]
    ncores = nb // c.B
    per = c.B * c.L
    hper = c.B * c.NT * c.KT * c.T
    in_maps = []
    for i in range(ncores):
        mi = dict(m)
        mi["xT"] = np.ascontiguousarray(m["xT"][:, i * hper:(i + 1) * hper])
        for l_ in range(c.DEPTH):
            mi["pT%d" % l_] = np.ascontiguousarray(m["pT%d" % l_][:, i * per:(i + 1) * per])
        in_maps.append(mi)
    res = run_bass_kernel_spmd(nc, in_maps, core_ids=list(range(ncores)), trace=trace)
    outs = [res.results[i]["outT"].reshape(P, c.B, c.NT, c.KT, c.T) for i in range(ncores)]
    o = np.concatenate(outs, axis=1)
    out = np.transpose(o, (1, 2, 4, 3, 0)).reshape(nb, c.L, c.D)
    return np.ascontiguousarray(out).astype(np.float32), res


def kernel(**inputs):
    c = Cfg(B=1)
    out, _ = run_cfg(c, inputs)
    return out
```
